# Optimizing a Trainium2 kernel written in Bass

```python
import jax, jax.numpy as jnp
from jax import lax
import numpy as np

D_MODEL = 2048
BATCH = 2
SEQ = 8192
DEPTH = 4

CHUNK = 64
Q_BLOCK = 128
N_MIXERS = 3
RMS_EPS = 1e-6
SB_HEADS = 16
SB_HEAD_DIM = D_MODEL // SB_HEADS
HG_HEADS = 16
HG_KEY_DIM = D_MODEL // HG_HEADS
HG_VAL_DIM = D_MODEL // HG_HEADS
HG_WIDTH = HG_HEADS * HG_KEY_DIM
MLA_HEADS = 16
MLA_NOPE = 128
MLA_ROPE = 64
MLA_V = 128
MLA_Q_RANK = 768
MLA_KV_RANK = 512
ROPE_THETA = 10000.0
D_FF = 4 * D_MODEL

kernel_name = "hybrid_stickbreak_hgrn2_mla_stream"


def _n_layers_of(m):
    return len(range(m, DEPTH, N_MIXERS))


def rms_norm(x, g):
    xf = x.astype(jnp.float32)
    y = xf * lax.rsqrt(jnp.mean(xf * xf, axis=-1, keepdims=True) + RMS_EPS)
    return (y * g.astype(jnp.float32)).astype(x.dtype)


def _sweep_query_blocks(block_fn, queries):
    B, S = queries[0].shape[:2]
    nb = S // Q_BLOCK
    blocked = tuple(a.reshape(B, nb, Q_BLOCK, *a.shape[2:]).swapaxes(0, 1) for a in queries)
    starts = jnp.arange(nb, dtype=jnp.int32) * Q_BLOCK
    out = lax.map(lambda xs: block_fn(xs[0], *xs[1]), (starts, blocked))
    return out.swapaxes(0, 1).reshape(B, S, *out.shape[3:])


def stick_breaking_mixer(h, w_qkv, w_o):
    B, S, _ = h.shape
    qkv = h @ w_qkv
    q, k, v = [a.reshape(B, S, SB_HEADS, SB_HEAD_DIM) for a in jnp.split(qkv, 3, axis=-1)]
    key_idx = jnp.arange(S)
    scale = SB_HEAD_DIM ** -0.5

    def block(q0, qb):
        z = jnp.einsum('bqhd,bkhd->bhqk', qb, k).astype(jnp.float32) * scale
        t = q0 + jnp.arange(Q_BLOCK)
        mask = key_idx[None, :] < t[:, None]
        log_one_minus = jnp.where(mask, -jax.nn.softplus(z), 0.0)
        after = lax.cumsum(log_one_minus, axis=3, reverse=True) - log_one_minus
        A = jnp.where(mask, jnp.exp(jax.nn.log_sigmoid(z) + after), 0.0)
        return jnp.einsum('bhqk,bkhd->bqhd', A.astype(v.dtype), v)

    o = _sweep_query_blocks(block, (q,))
    return o.reshape(B, S, SB_HEADS * SB_HEAD_DIM) @ w_o


def hgrn2_mixer(h, w_in, g_norm, w_o, lb):
    B, S, _ = h.shape
    N = S // CHUNK
    proj = h @ w_in
    q, fz, i, g = jnp.split(proj, [HG_WIDTH, 2 * HG_WIDTH, 2 * HG_WIDTH + HG_HEADS * HG_VAL_DIM], axis=-1)
    fz = fz.astype(jnp.float32)
    log_f = jnp.logaddexp(jnp.log(lb), jnp.log1p(-lb) + jax.nn.log_sigmoid(fz))
    k = (1.0 - lb) * jax.nn.sigmoid(-fz)

    def to_chunks(a, d):
        return a.astype(jnp.float32).reshape(B, N, CHUNK, HG_HEADS, d).transpose(1, 0, 3, 2, 4)

    xs = (to_chunks(q, HG_KEY_DIM), to_chunks(k, HG_KEY_DIM),
          to_chunks(i, HG_VAL_DIM), to_chunks(log_f, HG_KEY_DIM))
    causal = jnp.tril(jnp.ones((CHUNK, CHUNK), dtype=bool))[None, None, :, :, None]

    def step(state, chunk):
        qc, kc, vc, lfc = chunk
        b = jnp.cumsum(lfc, axis=2)
        b_last = b[:, :, -1:, :]
        o_inter = jnp.einsum('bhtc,bhcv->bhtv', qc * jnp.exp(b), state)
        diff = jnp.where(causal, b[:, :, :, None, :] - b[:, :, None, :, :], -jnp.inf)
        scores = jnp.einsum('bhtc,bhsc,bhtsc->bhts', qc, kc, jnp.exp(diff))
        o_intra = jnp.einsum('bhts,bhsv->bhtv', scores, vc)
        new_state = (jnp.exp(b_last[:, :, 0, :])[..., None] * state
                     + jnp.einsum('bhsc,bhsv->bhcv', kc * jnp.exp(b_last - b), vc))
        return new_state, o_inter + o_intra

    state0 = jnp.zeros((B, HG_HEADS, HG_KEY_DIM, HG_VAL_DIM), jnp.float32)
    _, o = lax.scan(step, state0, xs)
    o = o.transpose(1, 0, 3, 2, 4).reshape(B, S, HG_HEADS, HG_VAL_DIM)
    o = o * lax.rsqrt(jnp.mean(o * o, axis=-1, keepdims=True) + RMS_EPS) * g_norm.astype(jnp.float32)
    o = o.reshape(B, S, HG_HEADS * HG_VAL_DIM) * jax.nn.silu(g.astype(jnp.float32))
    return o.astype(h.dtype) @ w_o


def _rope_cos_sin(positions):
    inv_freq = ROPE_THETA ** (-jnp.arange(0, MLA_ROPE, 2, dtype=jnp.float32) / MLA_ROPE)
    ang = positions.astype(jnp.float32)[..., None] * inv_freq
    return jnp.cos(ang), jnp.sin(ang)


def _apply_rope(x, cos, sin):
    x1, x2 = jnp.split(x.astype(jnp.float32), 2, axis=-1)
    return jnp.concatenate([x1 * cos - x2 * sin, x1 * sin + x2 * cos], axis=-1).astype(x.dtype)


def mla_mixer(h, positions, w_dkv, q_norm, kv_norm, w_uq, w_ukv, w_o):
    B, S, _ = h.shape
    c_q, c_kv, k_pe = jnp.split(h @ w_dkv, [MLA_Q_RANK, MLA_Q_RANK + MLA_KV_RANK], axis=-1)
    c_q = rms_norm(c_q, q_norm)
    c_kv = rms_norm(c_kv, kv_norm)
    q = (c_q @ w_uq).reshape(B, S, MLA_HEADS, MLA_NOPE + MLA_ROPE)
    q_nope, q_pe = q[..., :MLA_NOPE], q[..., MLA_NOPE:]
    kv = (c_kv @ w_ukv).reshape(B, S, MLA_HEADS, MLA_NOPE + MLA_V)
    k_nope, v = kv[..., :MLA_NOPE], kv[..., MLA_NOPE:]
    cos, sin = _rope_cos_sin(positions)
    q_pe = _apply_rope(q_pe, cos[:, :, None, :], sin[:, :, None, :])
    k_pe = _apply_rope(k_pe, cos, sin)
    key_chunk = jnp.arange(S) // CHUNK
    scale = (MLA_NOPE + MLA_ROPE) ** -0.5

    def block(q0, qn, qp):
        s = (jnp.einsum('bqhd,bkhd->bhqk', qn, k_nope)
             + jnp.einsum('bqhr,bkr->bhqk', qp, k_pe)).astype(jnp.float32) * scale
        q_chunk = (q0 + jnp.arange(Q_BLOCK)) // CHUNK
        mask = key_chunk[None, :] <= q_chunk[:, None]
        p = jax.nn.softmax(jnp.where(mask, s, -jnp.inf), axis=-1)
        return jnp.einsum('bhqk,bkhd->bqhd', p.astype(v.dtype), v)

    o = _sweep_query_blocks(block, (q_nope, q_pe))
    return o.reshape(B, S, MLA_HEADS * MLA_V) @ w_o


def sq_relu_mlp(h, w1, w2):
    return jnp.square(jax.nn.relu(h @ w1)) @ w2


def setup_inputs(seed: int = 0) -> dict:
    key = jax.random.key(seed)
    ks = jax.random.split(key, 20)
    n_sb, n_hg, n_mla = _n_layers_of(0), _n_layers_of(1), _n_layers_of(2)

    def w(k, shape, fan_in):
        return jax.random.normal(k, shape, jnp.float32) * (fan_in ** -0.5)

    def gain(k, shape):
        return 1.0 + 0.02 * jax.random.normal(k, shape, jnp.float32)

    return {
        "x": jax.random.normal(ks[0], (BATCH, SEQ, D_MODEL), jnp.float32),
        "positions": jnp.broadcast_to(jnp.arange(SEQ, dtype=jnp.int32), (BATCH, SEQ)),
        "norm_mix": gain(ks[1], (DEPTH, D_MODEL)),
        "norm_mlp": gain(ks[2], (DEPTH, D_MODEL)),
        "final_norm": gain(ks[3], (D_MODEL,)),
        "sb_w_qkv": w(ks[4], (n_sb, D_MODEL, 3 * SB_HEADS * SB_HEAD_DIM), D_MODEL),
        "sb_w_o": w(ks[5], (n_sb, SB_HEADS * SB_HEAD_DIM, D_MODEL), SB_HEADS * SB_HEAD_DIM),
        "hg_w_in": w(ks[6], (n_hg, D_MODEL, 3 * HG_WIDTH + HG_HEADS * HG_VAL_DIM), D_MODEL),
        "hg_lb_logits": 0.5 * jax.random.normal(ks[7], (DEPTH, HG_WIDTH), jnp.float32),
        "hg_g_norm": gain(ks[8], (n_hg, HG_VAL_DIM)),
        "hg_w_o": w(ks[9], (n_hg, HG_HEADS * HG_VAL_DIM, D_MODEL), HG_HEADS * HG_VAL_DIM),
        "mla_w_dkv": w(ks[10], (n_mla, D_MODEL, MLA_Q_RANK + MLA_KV_RANK + MLA_ROPE), D_MODEL),
        "mla_q_norm": gain(ks[11], (n_mla, MLA_Q_RANK)),
        "mla_kv_norm": gain(ks[12], (n_mla, MLA_KV_RANK)),
        "mla_w_uq": w(ks[13], (n_mla, MLA_Q_RANK, MLA_HEADS * (MLA_NOPE + MLA_ROPE)), MLA_Q_RANK),
        "mla_w_ukv": w(ks[14], (n_mla, MLA_KV_RANK, MLA_HEADS * (MLA_NOPE + MLA_V)), MLA_KV_RANK),
        "mla_w_o": w(ks[15], (n_mla, MLA_HEADS * MLA_V, D_MODEL), MLA_HEADS * MLA_V),
        "mlp_w1": w(ks[16], (DEPTH, D_MODEL, D_FF), D_MODEL),
        "mlp_w2": w(ks[17], (DEPTH, D_FF, D_MODEL), D_FF),
    }


def reference(x, positions, norm_mix, norm_mlp, final_norm, sb_w_qkv, sb_w_o, hg_w_in,
              hg_lb_logits, hg_g_norm, hg_w_o, mla_w_dkv, mla_q_norm, mla_kv_norm,
              mla_w_uq, mla_w_ukv, mla_w_o, mlp_w1, mlp_w2):
    p_lb = jax.nn.softmax(hg_lb_logits.astype(jnp.float32), axis=0)
    lb_all = jnp.cumsum(p_lb, axis=0) - p_lb[0]
    h = x
    for i in range(DEPTH):
        m, j = i % N_MIXERS, i // N_MIXERS
        a = rms_norm(h, norm_mix[i])
        if m == 0:
            y = stick_breaking_mixer(a, sb_w_qkv[j], sb_w_o[j])
        elif m == 1:
            y = hgrn2_mixer(a, hg_w_in[j], hg_g_norm[j], hg_w_o[j], lb_all[i])
        else:
            y = mla_mixer(a, positions, mla_w_dkv[j], mla_q_norm[j], mla_kv_norm[j],
                          mla_w_uq[j], mla_w_ukv[j], mla_w_o[j])
        h = h + y
        h = h + sq_relu_mlp(rms_norm(h, norm_mlp[i]), mlp_w1[i], mlp_w2[i])
    return rms_norm(h, final_norm)
```

```python
import numpy as np
import concourse.bass as bass
import concourse.mybir as mybir
from concourse.bass_utils import run_bass_kernel_spmd

F32 = mybir.dt.float32
BF16 = mybir.dt.bfloat16
I32 = mybir.dt.int32
ALU = mybir.AluOpType
AF = mybir.ActivationFunctionType

ENGS = ("pe", "act", "dve", "pool", "sp")
NRING = 6
D = 2048
NT = 2048
TT = 1024
S_LEN = 8192
DFF = 8192
EPS = 1e-6


def I(m, *a, **k):
    return lambda h: getattr(h, m)(*a, **k)


class Sched:
    def __init__(self, nc):
        self.nc = nc
        self.q = {e: [] for e in ENGS}
        self.cnt = {e: 0 for e in ENGS}
        self.waited = {e: {} for e in ENGS}
        self.lastw = {}
        self.reads = {}
        self.sems = {}
        self.ndma = {e: 0 for e in ENGS}
        self.rr = 0

    def _sem(self, key):
        if key not in self.sems:
            nm = "s_" + "_".join(str(k) for k in (key if isinstance(key, tuple) else (key,)))
            self.sems[key] = self.nc.alloc_semaphore(nm)
        return self.sems[key]

    def _deps(self, reads, writes):
        deps = []
        for b in reads:
            if b in self.lastw:
                deps.append(self.lastw[b])
        for b in writes:
            if b in self.lastw:
                deps.append(self.lastw[b])
            deps.extend(self.reads.get(b, ()))
        return deps

    def _emit_waits(self, eng, deps, skip_same=False):
        best = {}
        for k, v in deps:
            if skip_same and k == eng:
                continue
            if v > best.get(k, 0):
                best[k] = v
        for k, v in best.items():
            if self.waited[eng].get(k, 0) >= v:
                continue
            self.waited[eng][k] = v
            self.q[eng].append(("wait", self._sem(k), v))

    def _commit(self, tok, reads, writes):
        for b in reads:
            self.reads.setdefault(b, []).append(tok)
        for b in writes:
            self.lastw[b] = tok
            self.reads[b] = []

    def op(self, eng, fn, reads=(), writes=(), skip_same=None):
        if skip_same is None:
            skip_same = eng == "pe"
        self._emit_waits(eng, self._deps(reads, writes), skip_same)
        self.cnt[eng] += 1
        tok = (eng, self.cnt[eng])
        self.q[eng].append(("op", fn, self._sem(eng), 1))
        self._commit(tok, reads, writes)
        return tok

    def dma(self, eng, fn, reads=(), writes=()):
        n = self.ndma[eng]
        self.ndma[eng] += 1
        slot, rnd = n % NRING, n // NRING
        key = ("d", eng, slot)
        deps = self._deps(reads, writes)
        if rnd > 0:
            deps.append((key, 16 * rnd))
        self._emit_waits(eng, deps)
        tok = (key, 16 * (rnd + 1))
        self.q[eng].append(("op", fn, self._sem(key), 16))
        self._commit(tok, reads, writes)
        return tok

    def wait_tokens(self, eng, toks):
        self._emit_waits(eng, list(toks))

    def emit(self):
        nc = self.nc
        names = {"pe": "tensor", "act": "scalar", "dve": "vector", "pool": "gpsimd", "sp": "sync"}
        with nc.Block() as block:
            for e in ENGS:
                items = self.q[e]
                if not items:
                    continue

                def body(h, items=items):
                    for it in items:
                        if it[0] == "wait":
                            h.wait_ge(it[1], it[2])
                        else:
                            it[1](h).then_inc(it[2], it[3])

                getattr(block, names[e])(body)


class Ctx:
    def __init__(self):
        self.nc = bass.Bass("TRN2", target_bir_lowering=False)
        self.S = Sched(self.nc)
        self.out_toks = []
        self.ins = {}

    def din(self, name, shape, dt=F32):
        if name not in self.ins:
            self.ins[name] = self.nc.dram_tensor(name, list(shape), dt, kind="ExternalInput").ap()
        return self.ins[name]

    def dout(self, name, shape, dt=F32):
        return self.nc.dram_tensor(name, list(shape), dt, kind="ExternalOutput").ap()

    def finish(self):
        self.S.wait_tokens("sp", self.out_toks)
        self.S.emit()
        return self.nc


class Chunked:
    def __init__(self, cx, name, R, C, dt, rpc, out=False):
        self.rpc, self.name, self.n = rpc, name, R // rpc
        mk = cx.dout if out else cx.din
        self.parts = [mk(f"{name}_{i}", [rpc, C], dt) for i in range(R // rpc)]

    def rows(self, r0, r1):
        i = r0 // self.rpc
        assert (r1 - 1) // self.rpc == i
        return self.parts[i][r0 - i * self.rpc:r1 - i * self.rpc, :]

    def kc(self, i):
        return self.parts[i].rearrange("(kc p) t -> p kc t", p=128)


class WPan:
    def __init__(self, cx, name, K, N):
        self.K, self.N, self.name, self.p = K, N, name, {}
        for kg in range((K + 2047) // 2048):
            kcg = (min(K, (kg + 1) * 2048) - kg * 2048) // 128
            for pn in range((N + 511) // 512):
                pw = min(512, N - pn * 512)
                self.p[(kg, pn)] = (cx.din(f"{name}_{kg}_{pn}", [128, kcg * pw]), kcg, pw)

    def panel(self, kg, pn):
        ap, kcg, pw = self.p[(kg, pn)]
        return ap.rearrange("p (kc n) -> p kc n", n=pw), kcg, pw


def _wpan_host(name, W):
    W = np.asarray(W, np.float32)
    K, N = W.shape
    out = {}
    for kg in range((K + 2047) // 2048):
        r1 = min(K, (kg + 1) * 2048)
        kcg = (r1 - kg * 2048) // 128
        for pn in range((N + 511) // 512):
            pw = min(512, N - pn * 512)
            blk = W[kg * 2048:r1, pn * 512:pn * 512 + pw].reshape(kcg, 128, pw).transpose(1, 0, 2)
            out[f"{name}_{kg}_{pn}"] = np.ascontiguousarray(blk).reshape(128, kcg * pw)
    return out


def _chunks_host(name, arr, rpc):
    return {f"{name}_{i}": np.ascontiguousarray(arr[i * rpc:(i + 1) * rpc]) for i in range(arr.shape[0] // rpc)}


class PhaseA:
    def __init__(self, cx, tag):
        nc = cx.nc
        self.cx, self.nc, self.S, self.tag = cx, nc, cx.S, tag
        a = nc.alloc_sbuf_tensor
        self.hT = a(tag + "hT", [128, 16, TT], F32)
        self.xn = a(tag + "xn", [128, 16, TT], BF16)
        self.uq = a(tag + "uq", [128, 16, TT], BF16)
        self.wp = [a(tag + f"wp{i}", [128, 16, 512], BF16) for i in range(3)]
        self.rstd = a(tag + "rstd", [128, TT], F32)
        self.tmp = [a(tag + f"tmp{i}", [128, 512], F32) for i in range(2)]
        self.ob = [a(tag + f"ob{i}", [128, 512], BF16) for i in range(4)]
        self.of = [a(tag + f"of{i}", [128, 512], F32) for i in range(2)]
        self.ones = a(tag + "ones", [128, 128], BF16)
        self.gains = a(tag + "gains", [128, 48], F32)
        self.ps = [nc.alloc_psum_tensor(tag + f"ps{i}", [128, 512], F32) for i in range(8)]
        self.nwp = 0
        self.nps = 0
        self.nob = 0
        self.nof = 0
        self.ntmp = 0
        self.nev = 0
        self.S.op("pool", I("memset", self.ones[:], 1.0), writes=["ones"])

    def k(self, name):
        return self.tag + name

    def load_panel(self, W, k0, KC, c0, pw):
        slot = self.nwp % 3
        self.nwp += 1
        wp = self.wp[slot]
        src, kcg, pw2 = W.panel(k0 // 2048, c0 // 512)
        assert kcg == KC and pw2 == pw and c0 % 512 == 0 and k0 % 2048 == 0
        self.S.dma("pool", I("dma_start", out=wp[:, 0:KC, 0:pw], in_=src), writes=[self.k(f"wp{slot}")])
        return wp, self.k(f"wp{slot}")

    def psum(self):
        i = self.nps % 8
        self.nps += 1
        return self.ps[i], self.k(f"ps{i}")

    def linear_fm(self, W, k0, KC, c0, ncols, x, xkey, evac, oc_base=0, xoff=0):
        S = self.S
        nsub = TT // 512
        for p0 in range(0, ncols, 512):
            pw = min(512, ncols - p0)
            wp, wkey = self.load_panel(W, k0, KC, c0 + p0, pw)
            for o0 in range(0, pw, 128):
                ow = min(128, pw - o0)
                for sub in range(nsub):
                    ps, pkey = self.psum()
                    for kc in range(KC):
                        S.op("pe", I("matmul",
                            ps[0:ow, :], lhsT=wp[:, kc, o0:o0 + ow], rhs=x[:, xoff + kc, sub * 512:(sub + 1) * 512],
                            start=(kc == 0), stop=(kc == KC - 1)),
                            reads=[wkey, xkey], writes=[pkey])
                    evac(oc_base + (p0 + o0) // 128, sub, ps, pkey, ow)

    def linear_tm(self, W, k0, KC, c0, ncols, x, xkey, evac, xoff=0):
        S = self.S
        for p0 in range(0, ncols, 512):
            wp, wkey = self.load_panel(W, k0, KC, c0 + p0, 512)
            for tb in range(TT // 128):
                ps, pkey = self.psum()
                for kc in range(KC):
                    S.op("pe", I("matmul",
                        ps[:, :], lhsT=x[:, xoff + kc, tb * 128:(tb + 1) * 128], rhs=wp[:, kc, 0:512],
                        start=(kc == 0), stop=(kc == KC - 1)),
                        reads=[wkey, xkey], writes=[pkey])
                evac(p0 // 512, tb, ps, pkey)

    def rmsnorm(self, gcol, out, outkey, out_f32=None, src_lo=0, nch=16, out_lo=0):
        S, hT = self.S, self.hT
        nsub = TT // 512
        sq = self.uq
        for kc in range(nch):
            S.op("act", I("activation", out=sq[:, kc, :], in_=hT[:, src_lo + kc, :], func=AF.Square),
                 reads=[self.k("hT")], writes=[self.k("uq")], skip_same=True)
        for sub in range(nsub):
            ps, pkey = self.psum()
            for kc in range(nch):
                S.op("pe", I("matmul",
                    ps[:, :], lhsT=self.ones[:, :], rhs=sq[:, kc, sub * 512:(sub + 1) * 512],
                    start=(kc == 0), stop=(kc == nch - 1)), reads=[self.k("uq"), "ones"], writes=[pkey])
            t = self.tmp[self.ntmp % 2]
            tkey = self.k(f"tmp{self.ntmp % 2}")
            self.ntmp += 1
            S.op("act", I("activation", out=t[:, :], in_=ps[:, :], func=AF.Ln, scale=1.0 / (nch * 128), bias=self.eps_ap()),
                 reads=[pkey, "ones"], writes=[tkey])
            S.op("act", I("activation", out=self.rstd[:, sub * 512:(sub + 1) * 512], in_=t[:, :], func=AF.Exp, scale=-0.5),
                 reads=[tkey], writes=[self.k("rstd")])
        for kc in range(nch):
            for sub in range(nsub):
                sl = slice(sub * 512, (sub + 1) * 512)
                if out_f32 is None:
                    S.op("dve", I("scalar_tensor_tensor",
                        out=out[:, out_lo + kc, sl], in0=hT[:, src_lo + kc, sl], scalar=self.gains[:, gcol + kc:gcol + kc + 1],
                        in1=self.rstd[:, sl], op0=ALU.mult, op1=ALU.mult),
                        reads=[self.k("hT"), self.k("rstd"), self.k("gains")], writes=[outkey], skip_same=True)
                else:
                    out_f32(kc, sub, sl)

    def eps_ap(self):
        return self.epsb[:, 0:1]


def _kc_view(ap2d):
    return ap2d.rearrange("(kc p) t -> p kc t", p=128)


def build_phaseA(cx, prev, nxt, tag="A"):
    nc, S = cx.nc, cx.S
    P = PhaseA(cx, tag)
    P.epsb = nc.alloc_sbuf_tensor(tag + "epsb", [128, 1], F32)
    S.op("pool", I("memset", P.epsb[:], EPS), writes=["ones"])
    hin = Chunked(cx, "h_in", D, NT, F32, 512)
    gains = cx.din("gains", [128, 48])
    S.dma("sp", I("dma_start", out=P.gains[:, :], in_=gains[:, :]), writes=[P.k("gains")])
    if prev is not None:
        o_in = Chunked(cx, "o_in", D, NT, BF16, 512)
        wo = WPan(cx, "w_o", D, D)
        w1 = WPan(cx, "w_1", D, DFF)
        w2 = WPan(cx, "w_2", DFF, D)
    kind = nxt
    if kind == "final":
        yout = Chunked(cx, "y_out", D, NT, F32, 512, out=True)
    else:
        hout = Chunked(cx, "h_out", D, NT, F32, 512, out=True)
    NK = 3 if kind == "sb" else 4
    if kind == "sb":
        wqkv = WPan(cx, "w_in", D, 3 * D)
    if kind == "hg":
        wqkv = WPan(cx, "w_in", D, 4 * D)
    if kind == "mla":
        wdkv = WPan(cx, "w_in", D, 1280)
        wkpe = [WPan(cx, "w_kpe", D, 64), WPan(cx, "w_kpes", D, 64)]
        wuq = WPan(cx, "w_uq", 768, 4096)
        wukv = WPan(cx, "w_ukv", 512, 4096)
        posb = cx.din("posb", [128, NT], I32)
        rconst = cx.din("rconst", [128, 4])
        P.rc = nc.alloc_sbuf_tensor(tag + "rc", [128, 4], F32)
        P.posi = nc.alloc_sbuf_tensor(tag + "posi", [128, TT], I32)
        P.dummy = nc.alloc_sbuf_tensor(tag + "dummy", [128, 8], F32)
        S.dma("sp", I("dma_start", out=P.rc[:, :], in_=rconst[:, :]), writes=[P.k("rc")])
    if kind in ("sb", "hg", "mla"):
        send = Chunked(cx, "send", 16 * NK * 128, NT, BF16, NK * 128, out=True)

    def add_to_h(oc, sub, ps, pkey, ow):
        sl = slice(sub * 512, (sub + 1) * 512)
        S.op("dve", I("tensor_tensor", out=P.hT[:, oc, sl], in0=ps[:, :], in1=P.hT[:, oc, sl], op=ALU.add),
             reads=[pkey, P.k("hT")], writes=[P.k("hT")], skip_same=True)

    for tt in range(NT // TT):
        tsl = slice(tt * TT, (tt + 1) * TT)
        for q4 in range(4):
            S.dma("sp", I("dma_start", out=P.hT[:, q4 * 4:(q4 + 1) * 4, :], in_=hin.kc(q4)[:, :, tsl]), writes=[P.k("hT"), P.k("hTk"), P.k("hTr")])
        if prev is not None:
            for q4 in range(4):
                S.dma("sp", I("dma_start", out=P.xn[:, q4 * 4:(q4 + 1) * 4, :], in_=o_in.kc(q4)[:, :, tsl]), writes=[P.k("xn")])
            P.linear_fm(wo, 0, 16, 0, D, P.xn, P.k("xn"), add_to_h)
            P.rmsnorm(0, P.xn, P.k("xn"))
            for q in range(4):
                def relu2(oc, sub, ps, pkey, ow):
                    sl = slice(sub * 512, (sub + 1) * 512)
                    t = P.tmp[P.ntmp % 2]
                    tkey = P.k(f"tmp{P.ntmp % 2}")
                    P.ntmp += 1
                    S.op("act", I("activation", out=t[:, :], in_=ps[:, :], func=AF.Relu), reads=[pkey], writes=[tkey])
                    S.op("dve", I("tensor_tensor", out=P.uq[:, oc, sl], in0=t[:, :], in1=t[:, :], op=ALU.mult),
                         reads=[tkey], writes=[P.k("uq")], skip_same=True)
                P.linear_fm(w1, 0, 16, q * 2048, 2048, P.xn, P.k("xn"), relu2)
                P.linear_fm(w2, q * 2048, 16, 0, D, P.uq, P.k("uq"), add_to_h)
        if kind == "final":
            def outf(kc, sub, sl):
                i = P.nof % 2
                P.nof += 1
                ob = P.of[i]
                S.op("dve", I("scalar_tensor_tensor",
                    out=ob[:, :], in0=P.hT[:, kc, sl], scalar=P.gains[:, 16 + kc:17 + kc],
                    in1=P.rstd[:, sl], op0=ALU.mult, op1=ALU.mult),
                    reads=[P.k("hT"), P.k("rstd"), P.k("gains")], writes=[P.k(f"of{i}")])
                t = S.dma("sp", I("dma_start", out=yout.rows(kc * 128, (kc + 1) * 128)[:, tt * TT + sub * 512: tt * TT + (sub + 1) * 512], in_=ob[:, :]),
                          reads=[P.k(f"of{i}")])
                cx.out_toks.append(t)
            P.rmsnorm(16, None, None, out_f32=outf)
            continue
        for q4 in range(4):
            t = S.dma("sp", I("dma_start", out=hout.kc(q4)[:, :, tsl], in_=P.hT[:, q4 * 4:(q4 + 1) * 4, :]), reads=[P.k("hT")])
            cx.out_toks.append(t)
        P.rmsnorm(16, P.xn, P.k("xn"))
        def to_ob(ps, pkey, rows=128):
            i = P.nob % 4
            P.nob += 1
            ob = P.ob[i]
            if P.nev % 2 == 0:
                S.op("act", I("activation", out=ob[0:rows, :], in_=ps[0:rows, :], func=AF.Copy), reads=[pkey], writes=[P.k(f"ob{i}")])
            else:
                S.op("dve", I("tensor_copy", out=ob[0:rows, :], in_=ps[0:rows, :]), reads=[pkey], writes=[P.k(f"ob{i}")])
            P.nev += 1
            return ob, P.k(f"ob{i}")

        def send_fm(ob, okey, head, which, sub, rows=128, roff=0, srow=0):
            j, hh = head // 4, head % 4
            r0 = ((j * 4 + hh) * NK + which) * 128 + roff
            t = S.dma("sp", I("dma_start", out=send.rows(r0, r0 + rows)[:, tt * TT + sub * 512: tt * TT + (sub + 1) * 512], in_=ob[srow:srow + rows, :]),
                      reads=[okey])
            cx.out_toks.append(t)

        def v_evac(j, tb, ps, pkey):
            ob, okey = to_ob(ps, pkey)
            tok0 = tt * TT + tb * 128
            for hh in range(4):
                r0 = ((j * 4 + hh) * NK + 2) * 128
                dst = send.rows(r0 + tok0 // 16, r0 + tok0 // 16 + 8).rearrange("r (a d) -> (r a) d", d=128)
                t = S.dma("sp", I("dma_start", out=dst, in_=ob[:, hh * 128:(hh + 1) * 128]), reads=[okey])
                cx.out_toks.append(t)

        if kind == "sb":
            def qk_evac(oc, sub, ps, pkey, ow):
                ob, okey = to_ob(ps, pkey)
                send_fm(ob, okey, oc % 16, oc // 16, sub)
            P.linear_fm(wqkv, 0, 16, 0, 2 * D, P.xn, P.k("xn"), qk_evac)
            P.linear_tm(wqkv, 0, 16, 2 * D, D, P.xn, P.k("xn"), v_evac)
        if kind == "hg":
            def qf_evac(oc, sub, ps, pkey, ow):
                ob, okey = to_ob(ps, pkey)
                send_fm(ob, okey, oc % 16, oc // 16, sub)
            P.linear_fm(wqkv, 0, 16, 0, 2 * D, P.xn, P.k("xn"), qf_evac)
            P.linear_tm(wqkv, 0, 16, 2 * D, D, P.xn, P.k("xn"), v_evac)

            def g_evac(oc, sub, ps, pkey, ow):
                ob, okey = to_ob(ps, pkey)
                send_fm(ob, okey, oc % 16, 3, sub)
            P.linear_fm(wqkv, 0, 16, 3 * D, D, P.xn, P.k("xn"), g_evac)
        if kind == "mla":
            hT = P.hT
            KH, KK, KR = P.k("hT"), P.k("hTk"), P.k("hTr")
            S.op("dve", I("memset", P.dummy[:, :], 0.0), writes=[KH, KK, KR, P.k("dummy")])

            def lat_evac(oc, sub, ps, pkey, ow):
                sl = slice(sub * 512, (sub + 1) * 512)
                if P.nev % 2 == 0:
                    S.op("act", I("activation", out=hT[:, oc, sl], in_=ps[:, :], func=AF.Copy), reads=[pkey], writes=[KH], skip_same=True)
                else:
                    S.op("dve", I("tensor_copy", out=hT[:, oc, sl], in_=ps[:, :]), reads=[pkey], writes=[KH], skip_same=True)
                P.nev += 1
            P.linear_fm(wdkv, 0, 16, 0, 1280, P.xn, P.k("xn"), lat_evac)
            for w in range(2):
                def kp_evac(oc, sub, ps, pkey, ow, w=w):
                    sl = slice(sub * 512, (sub + 1) * 512)
                    S.op("dve", I("tensor_copy", out=hT[0:64, 10 + w, sl], in_=ps[0:64, :]), reads=[pkey], writes=[KK], skip_same=True)
                P.linear_fm(wkpe[w], 0, 16, 0, 64, P.xn, P.k("xn"), kp_evac)
            P.rmsnorm(32, P.xn, P.k("xn"), src_lo=0, nch=6, out_lo=0)
            P.rmsnorm(38, P.xn, P.k("xn"), src_lo=6, nch=4, out_lo=6)
            S.dma("sp", I("dma_start", out=P.posi[:, :], in_=posb[:, tsl]), writes=[P.k("posi")])
            S.op("dve", I("tensor_copy", out=hT[:, 12, :], in_=P.posi[:, :]), reads=[P.k("posi")], writes=[KR])
            MAGIC = 12582912.0
            for (dst, shift) in ((14, 0.0), (13, 0.25)):
                S.op("dve", I("tensor_scalar", out=hT[:, 15, :], in0=hT[:, 12, :], scalar1=P.rc[:, 0:1], scalar2=shift, op0=ALU.mult, op1=ALU.add),
                     reads=[KR, P.k("rc")], writes=[KR])
                S.op("dve", I("tensor_scalar", out=hT[:, dst, :], in0=hT[:, 15, :], scalar1=MAGIC, scalar2=None, op0=ALU.add), reads=[KR], writes=[KR])
                S.op("dve", I("tensor_scalar", out=hT[:, dst, :], in0=hT[:, dst, :], scalar1=-MAGIC, scalar2=None, op0=ALU.add), reads=[KR], writes=[KR])
                S.op("dve", I("tensor_tensor", out=hT[:, 15, :], in0=hT[:, 15, :], in1=hT[:, dst, :], op=ALU.subtract), reads=[KR], writes=[KR])
                S.op("act", I("activation", out=hT[:, dst, :], in_=hT[:, 15, :], func=AF.Sin, scale=2.0 * np.pi), reads=[KR], writes=[KR])
            S.op("dve", I("tensor_scalar", out=hT[:, 14, :], in0=hT[:, 14, :], scalar1=P.rc[:, 1:2], scalar2=None, op0=ALU.mult), reads=[KR, P.k("rc")], writes=[KR])

            def q_evac(oc, sub, ps, pkey, ow):
                sl = slice(sub * 512, (sub + 1) * 512)
                if oc < 16:
                    ob, okey = to_ob(ps, pkey)
                    send_fm(ob, okey, oc, 0, sub)
                    return
                r, swapped = (oc - 16) // 2, (oc - 16) % 2
                t = P.tmp[sub]
                tkey = P.k(f"tmp{sub}")
                if not swapped:
                    S.op("dve", I("tensor_tensor", out=t[:, :], in0=ps[:, :], in1=hT[:, 13, sl], op=ALU.mult), reads=[pkey, KR], writes=[tkey])
                else:
                    f = P.of[sub]
                    fkey = P.k(f"of{sub}")
                    S.op("dve", I("tensor_tensor", out=f[:, :], in0=ps[:, :], in1=hT[:, 14, sl], op=ALU.mult), reads=[pkey, KR], writes=[fkey])
                    i = P.nob % 4
                    P.nob += 1
                    ob = P.ob[i]
                    S.op("dve", I("tensor_tensor", out=ob[:, :], in0=t[:, :], in1=f[:, :], op=ALU.add), reads=[tkey, fkey], writes=[P.k(f"ob{i}")])
                    send_fm(ob, P.k(f"ob{i}"), 2 * r, 3, sub, rows=64, roff=0, srow=0)
                    send_fm(ob, P.k(f"ob{i}"), 2 * r + 1, 3, sub, rows=64, roff=0, srow=64)
            P.linear_fm(wuq, 0, 6, 0, 4096, P.xn, P.k("xn"), q_evac)
            for sub in range(TT // 512):
                sl = slice(sub * 512, (sub + 1) * 512)
                t, tkey = P.tmp[sub], P.k(f"tmp{sub}")
                f, fkey = P.of[sub], P.k(f"of{sub}")
                S.op("dve", I("tensor_tensor", out=t[0:64, :], in0=hT[0:64, 10, sl], in1=hT[0:64, 13, sl], op=ALU.mult), reads=[KK, KR], writes=[tkey])
                S.op("dve", I("tensor_tensor", out=f[0:64, :], in0=hT[0:64, 11, sl], in1=hT[0:64, 14, sl], op=ALU.mult), reads=[KK, KR], writes=[fkey])
                i = P.nob % 4
                P.nob += 1
                ob = P.ob[i]
                S.op("dve", I("tensor_tensor", out=ob[0:64, :], in0=t[0:64, :], in1=f[0:64, :], op=ALU.add), reads=[tkey, fkey], writes=[P.k(f"ob{i}")])
                for head in range(16):
                    send_fm(ob, P.k(f"ob{i}"), head, 3, sub, rows=64, roff=64, srow=0)

            def kn_evac(oc, sub, ps, pkey, ow):
                ob, okey = to_ob(ps, pkey)
                send_fm(ob, okey, oc, 1, sub)
            P.linear_fm(wukv, 0, 4, 0, 2048, P.xn, P.k("xn"), kn_evac, xoff=6)
            P.linear_tm(wukv, 0, 4, 2048, 2048, P.xn, P.k("xn"), v_evac, xoff=6)
    return cx.finish()


def build_phaseB_sb(cx, tag="B"):
    nc, S = cx.nc, cx.S
    a = nc.alloc_sbuf_tensor
    recv = Chunked(cx, "recv", 4 * 12 * 128, NT, BF16, 384)
    consts = cx.din("sbc", [128, 256 + 4 * 512], F32)
    sendb = Chunked(cx, "sendb", 4 * 4 * 128, NT, BF16, 512, out=True)
    qT = [a(tag + f"qT{s}", [128, S_LEN], BF16) for s in range(2)]
    kT = [a(tag + f"kT{s}", [128, S_LEN], BF16) for s in range(2)]
    vv = [a(tag + f"v{s}", [128, 64, 128], BF16) for s in range(2)]
    cf = a(tag + "cf", [128, 256 + 2048], F32)
    cb = a(tag + "cb", [128, 256], BF16)
    mk = a(tag + "mk", [128, 2048], BF16)
    NE, NSP, NEE, NA = 6, 4, 3, 3
    eb = [a(tag + f"e{i}", [128, 512], F32) for i in range(NE)]
    spb = [a(tag + f"sp{i}", [128, 512], BF16) for i in range(NSP)]
    Eb = [a(tag + f"E{i}", [128, 512], F32) for i in range(NEE)]
    Ab = [a(tag + f"A{i}", [128, 512], BF16) for i in range(NA)]
    ob = [a(tag + f"o{i}", [128, 512], BF16) for i in range(2)]
    Z = [nc.alloc_psum_tensor(tag + f"Z{i}", [128, 512], F32) for i in range(2)]
    AFp = [nc.alloc_psum_tensor(tag + f"AF{i}", [128, 512], F32) for i in range(2)]
    O = [nc.alloc_psum_tensor(tag + f"O{i}", [128, 512], F32) for i in range(4)]
    S.dma("sp", I("dma_start", out=cf[:, :], in_=consts[:, :]), writes=["cf"])
    S.op("dve", I("tensor_copy", out=cb[:, :], in_=cf[:, 0:256]), reads=["cf"], writes=["cb"])
    S.op("dve", I("tensor_copy", out=mk[:, :], in_=cf[:, 256:256 + 2048]), reads=["cf"], writes=["mk"])
    scale = 128.0 ** -0.5
    nob = [0]

    for pair in range(2):
        for s in range(2):
            hh = pair * 2 + s
            for src in range(4):
                r0 = (src * 12 + hh * 3) * 128
                S.dma("sp", I("dma_start", out=qT[s][:, src * NT:(src + 1) * NT], in_=recv.rows(r0, r0 + 128)), writes=[f"qT{s}"])
                S.dma("sp", I("dma_start", out=kT[s][:, src * NT:(src + 1) * NT], in_=recv.rows(r0 + 128, r0 + 256)), writes=[f"kT{s}"])
                vsrc = recv.rows(r0 + 256, r0 + 384).rearrange("r (a d) -> (r a) d", d=128).rearrange("(blk p) d -> p blk d", p=128)
                S.dma("sp", I("dma_start", out=vv[s][:, src * 16:(src + 1) * 16, :], in_=vsrc), writes=[f"v{s}"])
        blocks = []
        for qt in range(16):
            kbs = list(range(4 * qt + 3, -1, -1))
            for idx, kb in enumerate(kbs):
                for s in range(2):
                    blocks.append((s, qt, kb, idx == 0, idx == len(kbs) - 1))
        N = len(blocks)

        def st_z(n):
            s, qt, kb, first, last = blocks[n]
            z = n % 2
            S.op("pe", I("matmul", Z[z][:, :], lhsT=kT[s][:, kb * 128:(kb + 1) * 128], rhs=qT[s][:, qt * 512:(qt + 1) * 512], start=True, stop=True),
                 reads=[f"kT{s}", f"qT{s}"], writes=[f"Z{z}"])

        def st_e(n):
            s, qt, kb, first, last = blocks[n]
            z, e = n % 2, n % NE
            S.op("act", I("activation", out=eb[e][:, :], in_=Z[z][:, :], func=AF.Exp, scale=scale), reads=[f"Z{z}"], writes=[f"e{e}"])
            dd = kb - 4 * qt
            if dd >= 0:
                S.op("dve", I("tensor_tensor", out=eb[e][:, :], in0=eb[e][:, :], in1=mk[:, dd * 512:(dd + 1) * 512], op=ALU.mult),
                     reads=[f"e{e}", "mk"], writes=[f"e{e}"])

        def st_sp(n):
            e, p = n % NE, n % NSP
            S.op("act", I("activation", out=spb[p][:, :], in_=eb[e][:, :], func=AF.Ln, bias=1.0, scale=1.0), reads=[f"e{e}"], writes=[f"sp{p}"])

        def st_U(n):
            s, qt, kb, first, last = blocks[n]
            p = n % NSP
            S.op("pe", I("matmul", AFp[s][:, :], lhsT=cb[:, 0:128], rhs=spb[p][:, :], start=first, stop=False, skip_group_check=True),
                 reads=[f"sp{p}", "cb"], writes=[f"AF{s}"])

        def st_E(n):
            s, qt, kb, first, last = blocks[n]
            ee = n % NEE
            S.op("act", I("activation", out=Eb[ee][:, :], in_=AFp[s][:, :], func=AF.Exp), reads=[f"AF{s}"], writes=[f"E{ee}"])

        def st_L(n):
            s, qt, kb, first, last = blocks[n]
            p = n % NSP
            if last:
                return
            S.op("pe", I("matmul", AFp[s][:, :], lhsT=cb[:, 128:256], rhs=spb[p][:, :], start=False, stop=False, skip_group_check=True),
                 reads=[f"sp{p}", "cb"], writes=[f"AF{s}"])

        def st_A(n):
            e, ee, aa = n % NE, n % NEE, n % NA
            S.op("dve", I("tensor_tensor", out=Ab[aa][:, :], in0=eb[e][:, :], in1=Eb[ee][:, :], op=ALU.mult),
                 reads=[f"e{e}", f"E{ee}"], writes=[f"A{aa}"])

        def st_AV(n):
            s, qt, kb, first, last = blocks[n]
            aa = n % NA
            oi = s * 2 + (qt % 2)
            S.op("pe", I("matmul", O[oi][:, :], lhsT=vv[s][:, kb, :], rhs=Ab[aa][:, :], start=first, stop=last),
                 reads=[f"A{aa}", f"v{s}"], writes=[f"O{oi}"])
            if last:
                i = nob[0] % 2
                nob[0] += 1
                S.op("dve", I("tensor_copy", out=ob[i][:, :], in_=O[oi][:, :]), reads=[f"O{oi}"], writes=[f"o{i}"])
                hh = pair * 2 + s
                dest, off = (qt * 512) // NT, (qt * 512) % NT
                r0 = (dest * 4 + hh) * 128
                t = S.dma("sp", I("dma_start", out=sendb.rows(r0, r0 + 128)[:, off:off + 512], in_=ob[i][:, :]), reads=[f"o{i}"])
                cx.out_toks.append(t)

        for step in range(-3, N + 3):
            for fn, off in ((st_z, 3), (st_U, 0), (st_L, -1), (st_AV, -2)):
                n = step + off
                if 0 <= n < N:
                    fn(n)
            for fn, off in ((st_e, 2), (st_sp, 1), (st_E, 0)):
                n = step + off
                if 0 <= n < N:
                    fn(n)
            n = step - 1
            if 0 <= n < N:
                st_A(n)
    return cx.finish()


def build_phaseB_mla(cx, tag="M"):
    nc, S = cx.nc, cx.S
    a = nc.alloc_sbuf_tensor
    recv = Chunked(cx, "recv", 16 * 4 * 128, NT, BF16, 512)
    consts = cx.din("mlc", [128, 4 * 512], F32)
    sendb = Chunked(cx, "sendb", 4 * 4 * 128, NT, BF16, 512, out=True)
    qn = [a(tag + f"qn{s}", [128, S_LEN], BF16) for s in range(2)]
    kn = [a(tag + f"kn{s}", [128, S_LEN], BF16) for s in range(2)]
    vv = [a(tag + f"v{s}", [128, 64, 128], BF16) for s in range(2)]
    qp = [a(tag + f"qp{s}", [64, S_LEN], BF16) for s in range(2)]
    kp = a(tag + "kp", [64, S_LEN], BF16)
    cf = a(tag + "cf", [128, 2048], F32)
    mk = a(tag + "mk", [128, 2048], BF16)
    ones = a(tag + "ones", [128, 128], BF16)
    NE, NA = 3, 4
    eb = [a(tag + f"e{i}", [128, 512], F32) for i in range(NE)]
    Ab = [a(tag + f"A{i}", [128, 512], BF16) for i in range(NA)]
    rec = a(tag + "rec", [128, 512], F32)
    ob = [a(tag + f"o{i}", [128, 512], BF16) for i in range(2)]
    Z = [nc.alloc_psum_tensor(tag + f"Z{i}", [128, 512], F32) for i in range(2)]
    DEN = [nc.alloc_psum_tensor(tag + f"DN{i}", [128, 512], F32) for i in range(2)]
    O = [nc.alloc_psum_tensor(tag + f"O{i}", [128, 512], F32) for i in range(4)]
    S.dma("sp", I("dma_start", out=cf[:, :], in_=consts[:, :]), writes=["cf"])
    S.op("dve", I("tensor_copy", out=mk[:, :], in_=cf[:, :]), reads=["cf"], writes=["mk"])
    S.op("pool", I("memset", ones[:, :], 1.0), writes=["ones"])
    scale = 192.0 ** -0.5
    nob = [0]
    for src in range(4):
        S.dma("sp", I("dma_start", out=kp[:, src * NT:(src + 1) * NT], in_=recv.rows(src * 2048 + 448, src * 2048 + 512)), writes=["kp"])

    for pair in range(2):
        for s in range(2):
            hh = pair * 2 + s
            for src in range(4):
                r0 = (src * 4 + hh) * 512
                S.dma("sp", I("dma_start", out=qn[s][:, src * NT:(src + 1) * NT], in_=recv.rows(r0, r0 + 128)), writes=[f"qn{s}"])
                S.dma("sp", I("dma_start", out=kn[s][:, src * NT:(src + 1) * NT], in_=recv.rows(r0 + 128, r0 + 256)), writes=[f"kn{s}"])
                vsrc = recv.rows(r0 + 256, r0 + 384).rearrange("r (a d) -> (r a) d", d=128).rearrange("(blk p) d -> p blk d", p=128)
                S.dma("sp", I("dma_start", out=vv[s][:, src * 16:(src + 1) * 16, :], in_=vsrc), writes=[f"v{s}"])
                S.dma("sp", I("dma_start", out=qp[s][:, src * NT:(src + 1) * NT], in_=recv.rows(r0 + 384, r0 + 448)), writes=[f"qp{s}"])
        blocks = []
        for qt in range(16):
            kbs = list(range(4 * qt + 3, -1, -1))
            for idx, kb in enumerate(kbs):
                for s in range(2):
                    blocks.append((s, qt, kb, idx == 0, idx == len(kbs) - 1))
        N = len(blocks)

        def st_z(n):
            s, qt, kb, first, last = blocks[n]
            z = n % 2
            S.op("pe", I("matmul", Z[z][:, :], lhsT=kn[s][:, kb * 128:(kb + 1) * 128], rhs=qn[s][:, qt * 512:(qt + 1) * 512], start=True, stop=False),
                 reads=[f"kn{s}", f"qn{s}"], writes=[f"Z{z}"])
            S.op("pe", I("matmul", Z[z][:, :], lhsT=kp[:, kb * 128:(kb + 1) * 128], rhs=qp[s][:, qt * 512:(qt + 1) * 512], start=False, stop=True),
                 reads=["kp", f"qp{s}"], writes=[f"Z{z}"])

        def st_e(n):
            s, qt, kb, first, last = blocks[n]
            z, e, aa = n % 2, n % NE, n % NA
            dd = kb - 4 * qt
            if dd >= 0:
                S.op("act", I("activation", out=eb[e][:, :], in_=Z[z][:, :], func=AF.Exp, scale=scale), reads=[f"Z{z}"], writes=[f"e{e}"])
                S.op("dve", I("tensor_tensor", out=Ab[aa][:, :], in0=eb[e][:, :], in1=mk[:, dd * 512:(dd + 1) * 512], op=ALU.mult),
                     reads=[f"e{e}", "mk"], writes=[f"A{aa}"])
            else:
                S.op("act", I("activation", out=Ab[aa][:, :], in_=Z[z][:, :], func=AF.Exp, scale=scale), reads=[f"Z{z}"], writes=[f"A{aa}"])

        def st_AV(n):
            s, qt, kb, first, last = blocks[n]
            aa = n % NA
            oi = s * 2 + (qt % 2)
            S.op("pe", I("matmul", O[oi][:, :], lhsT=vv[s][:, kb, :], rhs=Ab[aa][:, :], start=first, stop=last),
                 reads=[f"A{aa}", f"v{s}"], writes=[f"O{oi}"])
            S.op("pe", I("matmul", DEN[s][:, :], lhsT=ones[:, :], rhs=Ab[aa][:, :], start=first, stop=last),
                 reads=[f"A{aa}", "ones"], writes=[f"DN{s}"])
            if last:
                i = nob[0] % 2
                nob[0] += 1
                S.op("dve", I("reciprocal", out=rec[:, :], in_=DEN[s][:, :]), reads=[f"DN{s}"], writes=["rec"])
                S.op("dve", I("tensor_tensor", out=ob[i][:, :], in0=O[oi][:, :], in1=rec[:, :], op=ALU.mult), reads=[f"O{oi}", "rec"], writes=[f"o{i}"])
                hh = pair * 2 + s
                dest, off = (qt * 512) // NT, (qt * 512) % NT
                r0 = (dest * 4 + hh) * 128
                t = S.dma("sp", I("dma_start", out=sendb.rows(r0, r0 + 128)[:, off:off + 512], in_=ob[i][:, :]), reads=[f"o{i}"])
                cx.out_toks.append(t)

        for step in range(-2, N + 2):
            for fn, off in ((st_z, 2), (st_AV, 0)):
                n = step + off
                if 0 <= n < N:
                    fn(n)
            n = step + 1
            if 0 <= n < N:
                st_e(n)
    return cx.finish()


def build_phaseB_hg(cx, layer_i=1, tag="H"):
    nc, S = cx.nc, cx.S
    a = nc.alloc_sbuf_tensor
    recv = Chunked(cx, "recv", 16 * 4 * 128, NT, BF16, 512)
    consts = cx.din("hgc", [128, 128 + 128 + 512], F32)
    lbl = cx.din("lbl", [128, 16])
    gnorm = cx.din("gnorm", [128, 1])
    sendb = Chunked(cx, "sendb", 4 * 4 * 128, NT, BF16, 512, out=True)
    SEG = NT
    qT = [a(tag + f"q{h}", [128, SEG], BF16) for h in range(4)]
    fz = [a(tag + f"fz{h}", [128, SEG], BF16) for h in range(4)]
    gT = [a(tag + f"g{h}", [128, SEG], BF16) for h in range(4)]
    itm = [a(tag + f"i{h}", [128, 16, 128], BF16) for h in range(4)]
    qb = [a(tag + f"qb{h}", [128, SEG], BF16) for h in range(4)]
    ke = [a(tag + f"ke{h}", [128, SEG], BF16) for h in range(4)]
    dec = [a(tag + f"dec{h}", [128, 32], F32) for h in range(4)]
    osg = [a(tag + f"os{h}", [128, SEG], F32) for h in range(4)]
    W = [a(tag + f"W{i}", [128, SEG], F32) for i in range(3)]
    sqb = a(tag + "sqb", [128, SEG], BF16)
    obf = a(tag + "obf", [128, SEG], BF16)
    st = [a(tag + f"st{h}", [128, 128], F32) for h in range(4)]
    stb = [a(tag + f"stb{h}", [128, 128], BF16) for h in range(4)]
    tmpS = [a(tag + f"ts{h}", [128, 128], F32) for h in range(4)]
    ketm = [[a(tag + f"kt{h}_{i}", [128, 128], BF16) for i in range(2)] for h in range(4)]
    scm = [[a(tag + f"sc{h}_{i}", [128, 128], BF16) for i in range(2)] for h in range(4)]
    cf = a(tag + "cf", [128, 768], F32)
    mkb = a(tag + "mkb", [128, 128], BF16)
    idb = a(tag + "idb", [128, 128], BF16)
    ones = a(tag + "ones", [128, 128], BF16)
    lbw = a(tag + "lbw", [128, 16], F32)
    lbs = a(tag + "lbs", [128, 16], F32)
    gn = a(tag + "gn", [128, 1], F32)
    epsb = a(tag + "epsb", [128, 1], F32)
    TP = [nc.alloc_psum_tensor(tag + f"TP{i}", [128, 128], BF16) for i in range(1)]
    SC = [nc.alloc_psum_tensor(tag + f"SC{i}", [128, 128], F32) for i in range(1)]
    OB = [nc.alloc_psum_tensor(tag + f"OB{i}", [128, 128], F32) for i in range(4)]
    UP = [nc.alloc_psum_tensor(tag + f"UP{i}", [128, 512], F32) for i in range(2)]
    cnt = {"tp": 0, "sc": 0, "ob": 0, "up": 0, "ev": 0}

    S.dma("sp", I("dma_start", out=cf[:, :], in_=consts[:, :]), writes=["cf"])
    S.dma("sp", I("dma_start", out=lbw[:, :], in_=lbl[:, :]), writes=["lbw"])
    S.dma("sp", I("dma_start", out=gn[:, :], in_=gnorm[:, :]), writes=["gn"])
    S.op("dve", I("tensor_copy", out=mkb[:, :], in_=cf[:, 0:128]), reads=["cf"], writes=["mkb"])
    S.op("dve", I("tensor_copy", out=idb[:, :], in_=cf[:, 128:256]), reads=["cf"], writes=["idb"])
    S.op("pool", I("memset", ones[:, :], 1.0), writes=["ones"])
    S.op("pool", I("memset", epsb[:, :], EPS), writes=["epsb"])
    for h in range(4):
        S.op("pool", I("memset", st[h][:, :], 0.0), writes=[f"st{h}"])
        S.op("pool", I("memset", stb[h][:, :], 0.0), writes=[f"stb{h}"])
    S.op("act", I("activation", out=lbw[:, :], in_=lbw[:, :], func=AF.Exp), reads=["lbw"], writes=["lbw"])
    for h in range(4):
        c0 = h * 4
        S.op("dve", I("tensor_reduce", out=lbs[:, c0:c0 + 1], in_=lbw[:, c0:c0 + 4], axis=mybir.AxisListType.X, op=ALU.add), reads=["lbw"], writes=["lbs"])
        S.op("dve", I("reciprocal", out=lbs[:, c0:c0 + 1], in_=lbs[:, c0:c0 + 1]), reads=["lbs"], writes=["lbs"])
        if layer_i >= 1:
            S.op("dve", I("tensor_reduce", out=lbs[:, c0 + 3:c0 + 4], in_=lbw[:, c0 + 1:c0 + 1 + layer_i], axis=mybir.AxisListType.X, op=ALU.add), reads=["lbw"], writes=["lbs"])
        else:
            S.op("dve", I("memset", lbs[:, c0 + 3:c0 + 4], 0.0), writes=["lbs"])
        S.op("dve", I("tensor_tensor", out=lbs[:, c0 + 1:c0 + 2], in0=lbs[:, c0 + 3:c0 + 4], in1=lbs[:, c0:c0 + 1], op=ALU.mult), reads=["lbs"], writes=["lbs"])
        S.op("dve", I("tensor_scalar", out=lbs[:, c0 + 2:c0 + 3], in0=lbs[:, c0 + 1:c0 + 2], scalar1=-1.0, scalar2=1.0, op0=ALU.mult, op1=ALU.add), reads=["lbs"], writes=["lbs"])

    def ring(name, n=2):
        i = cnt[name] % n
        cnt[name] += 1
        return i

    for seg in range(4):
        for h in range(4):
            r0 = (seg * 4 + h) * 512
            S.dma("sp", I("dma_start", out=qT[h][:, :], in_=recv.rows(r0, r0 + 128)), writes=[f"q{h}"])
            S.dma("sp", I("dma_start", out=fz[h][:, :], in_=recv.rows(r0 + 128, r0 + 256)), writes=[f"fz{h}"])
            isrc = recv.rows(r0 + 256, r0 + 384).rearrange("r (a d) -> (r a) d", d=128).rearrange("(blk p) d -> p blk d", p=128)
            S.dma("sp", I("dma_start", out=itm[h][:, :, :], in_=isrc), writes=[f"i{h}"])
            S.dma("sp", I("dma_start", out=gT[h][:, :], in_=recv.rows(r0 + 384, r0 + 512)), writes=[f"g{h}"])
        for h in range(4):
            c0 = h * 4
            W0, W1, W2 = W
            S.op("act", I("activation", out=W0[:, :], in_=fz[h][:, :], func=AF.Exp, scale=-1.0), reads=[f"fz{h}"], writes=["W0"])
            S.op("dve", I("tensor_scalar", out=W0[:, :], in0=W0[:, :], scalar1=1.0, scalar2=None, op0=ALU.add), reads=["W0"], writes=["W0"])
            S.op("dve", I("reciprocal", out=W0[:, :], in_=W0[:, :]), reads=["W0"], writes=["W0"])
            S.op("dve", I("tensor_scalar", out=W1[:, :], in0=W0[:, :], scalar1=lbs[:, c0 + 2:c0 + 3], scalar2=lbs[:, c0 + 1:c0 + 2], op0=ALU.mult, op1=ALU.add),
                 reads=["W0", "lbs"], writes=["W1"])
            S.op("act", I("activation", out=W2[:, :], in_=W1[:, :], func=AF.Ln), reads=["W1"], writes=["W2"])
            S.op("dve", I("tensor_scalar", out=W0[:, :], in0=W1[:, :], scalar1=-1.0, scalar2=1.0, op0=ALU.mult, op1=ALU.add), reads=["W1"], writes=["W0"])
            for j in range(SEG // 512):
                sl = slice(j * 512, (j + 1) * 512)
                S.op("dve", I("tensor_tensor_scan", out=W1[:, sl], data0=cf[:, 256:768], data1=W2[:, sl], initial=0.0, op0=ALU.mult, op1=ALU.add),
                     reads=["W2", "cf", "W0"], writes=["W1"])
            bend = W1[:, :].rearrange("p (n c) -> p n c", c=64)[:, :, 63]
            S.op("act", I("activation", out=dec[h][:, :], in_=bend, func=AF.Exp), reads=["W1"], writes=[f"dec{h}"])
            S.op("act", I("activation", out=W2[:, :], in_=W1[:, :], func=AF.Exp), reads=["W1"], writes=["W2"])
            S.op("dve", I("tensor_tensor", out=qb[h][:, :], in0=qT[h][:, :], in1=W2[:, :], op=ALU.mult), reads=[f"q{h}", "W2"], writes=[f"qb{h}"])
            S.op("act", I("activation", out=W2[:, :], in_=W1[:, :], func=AF.Exp, scale=-1.0), reads=["W1", f"qb{h}"], writes=["W2"])
            S.op("dve", I("tensor_tensor", out=ke[h][:, :], in0=W0[:, :], in1=W2[:, :], op=ALU.mult), reads=["W0", "W2"], writes=[f"ke{h}"])

        def phase1(blk):
            for h in range(4):
                bsl = slice(blk * 128, (blk + 1) * 128)
                r = blk % 2
                tp = ring("tp", 1)
                S.op("pe", I("transpose", TP[tp][:, :], ke[h][:, bsl], idb[:, :]), reads=[f"ke{h}", "idb"], writes=[f"TP{tp}"])
                if cnt["ev"] % 2 == 0:
                    S.op("act", I("activation", out=ketm[h][r][:, :], in_=TP[tp][:, :], func=AF.Copy), reads=[f"TP{tp}"], writes=[f"kt{h}_{r}"])
                else:
                    S.op("dve", I("tensor_copy", out=ketm[h][r][:, :], in_=TP[tp][:, :]), reads=[f"TP{tp}"], writes=[f"kt{h}_{r}"])
                cnt["ev"] += 1
                sc = ring("sc", 1)
                S.op("pe", I("matmul", SC[sc][:, :], lhsT=ke[h][:, bsl], rhs=qb[h][:, bsl], start=True, stop=True), reads=[f"ke{h}", f"qb{h}"], writes=[f"SC{sc}"])
                S.op("dve", I("tensor_tensor", out=scm[h][r][:, :], in0=SC[sc][:, :], in1=mkb[:, :], op=ALU.mult), reads=[f"SC{sc}", "mkb"], writes=[f"sc{h}_{r}"])

        def phase2(blk):
            obs = {}
            r = blk % 2
            for h2 in range(2):
                for h in range(4):
                    if h2 == 0:
                        obs[h] = h
                        S.op("pe", I("matmul", OB[obs[h]][:, :], lhsT=itm[h][:, blk, :], rhs=scm[h][r][:, :], start=True, stop=False),
                             reads=[f"i{h}", f"sc{h}_{r}"], writes=[f"OB{obs[h]}"])
                    ob = obs[h]
                    csl = slice(blk * 128 + h2 * 64, blk * 128 + (h2 + 1) * 64)
                    S.op("pe", I("matmul", OB[ob][:, h2 * 64:(h2 + 1) * 64], lhsT=stb[h][:, :], rhs=qb[h][:, csl], start=False, stop=(h2 == 1)),
                         reads=[f"stb{h}", f"qb{h}"], writes=[f"OB{ob}"])
                    up = ring("up")
                    psl = slice(h2 * 64, (h2 + 1) * 64)
                    S.op("pe", I("matmul", UP[up][:, 0:128], lhsT=ketm[h][r][psl, :], rhs=itm[h][psl, blk, :], start=True, stop=True),
                         reads=[f"kt{h}_{r}", f"i{h}"], writes=[f"UP{up}"])
                    dcol = dec[h][:, 2 * blk + h2:2 * blk + h2 + 1]
                    S.op("dve", I("tensor_scalar", out=tmpS[h][:, :], in0=st[h][:, :], scalar1=dcol, scalar2=None, op0=ALU.mult),
                         reads=[f"st{h}", f"dec{h}"], writes=[f"ts{h}"])
                    S.op("dve", I("scalar_tensor_tensor", out=st[h][:, :], in0=UP[up][:, 0:128], scalar=dcol, in1=tmpS[h][:, :], op0=ALU.mult, op1=ALU.add),
                         reads=[f"UP{up}", f"ts{h}", f"dec{h}"], writes=[f"st{h}"])
                    S.op("act", I("activation", out=stb[h][:, :], in_=st[h][:, :], func=AF.Copy), reads=[f"st{h}"], writes=[f"stb{h}"])
                    if h2 == 1:
                        S.op("act", I("activation", out=osg[h][:, blk * 128:(blk + 1) * 128], in_=OB[ob][:, :], func=AF.Copy),
                             reads=[f"OB{ob}"], writes=[f"os{h}"])

        phase1(0)
        for blk in range(16):
            if blk + 1 < 16:
                phase1(blk + 1)
            phase2(blk)

        for h in range(4):
            W0, W1, W2 = W
            S.op("act", I("activation", out=sqb[:, :], in_=osg[h][:, :], func=AF.Square), reads=[f"os{h}"], writes=["sqb"])
            for j in range(SEG // 512):
                sl = slice(j * 512, (j + 1) * 512)
                up = ring("up")
                S.op("pe", I("matmul", UP[up][:, :], lhsT=ones[:, :], rhs=sqb[:, sl], start=True, stop=True), reads=["sqb", "ones"], writes=[f"UP{up}"])
                S.op("act", I("activation", out=W0[:, sl], in_=UP[up][:, :], func=AF.Ln, scale=1.0 / 128, bias=epsb[:, 0:1]), reads=[f"UP{up}", "epsb"], writes=["W0"])
            S.op("act", I("activation", out=W0[:, :], in_=W0[:, :], func=AF.Exp, scale=-0.5), reads=["W0"], writes=["W0"])
            S.op("act", I("activation", out=W1[:, :], in_=gT[h][:, :], func=AF.Exp, scale=-1.0), reads=[f"g{h}"], writes=["W1"])
            S.op("dve", I("tensor_scalar", out=W1[:, :], in0=W1[:, :], scalar1=1.0, scalar2=None, op0=ALU.add), reads=["W1"], writes=["W1"])
            S.op("dve", I("reciprocal", out=W1[:, :], in_=W1[:, :]), reads=["W1"], writes=["W1"])
            S.op("dve", I("tensor_tensor", out=W2[:, :], in0=gT[h][:, :], in1=W1[:, :], op=ALU.mult), reads=[f"g{h}", "W1"], writes=["W2"])
            S.op("dve", I("scalar_tensor_tensor", out=W0[:, :], in0=osg[h][:, :], scalar=gn[:, 0:1], in1=W0[:, :], op0=ALU.mult, op1=ALU.mult),
                 reads=[f"os{h}", "gn", "W0"], writes=["W0"])
            S.op("dve", I("tensor_tensor", out=obf[:, :], in0=W0[:, :], in1=W2[:, :], op=ALU.mult), reads=["W0", "W2"], writes=["obf"])
            r0 = (seg * 4 + h) * 128
            t = S.dma("sp", I("dma_start", out=sendb.rows(r0, r0 + 128), in_=obf[:, :]), reads=["obf"])
            cx.out_toks.append(t)
    return cx.finish()


def _sb_consts():
    j = np.arange(128)[:, None]
    s = np.arange(128)[None, :]
    Uneg = -(j >= s).astype(np.float32)
    Lneg = -(j < s).astype(np.float32)
    masks = []
    t = np.arange(512)[None, :]
    for dd in range(4):
        masks.append((t > (j + 128 * dd)).astype(np.float32))
    return np.concatenate([Uneg, Lneg] + masks, axis=1).astype(np.float32)


def _hg_consts():
    j = np.arange(128)[:, None]
    t = np.arange(128)[None, :]
    mask = ((j // 64 == t // 64) & (j <= t)).astype(np.float32)
    ident = np.eye(128, dtype=np.float32)
    reset = np.ones((128, 512), np.float32)
    reset[:, ::64] = 0.0
    return np.concatenate([mask, ident, reset], axis=1).astype(np.float32)


def _mla_consts():
    j = np.arange(128)[:, None]
    t = np.arange(512)[None, :]
    return np.concatenate([((2 * dd + j // 64) <= (t // 64)).astype(np.float32) for dd in range(4)], axis=1).astype(np.float32)


def _rope_consts():
    p = np.arange(128)
    inv_freq = (10000.0 ** (-np.arange(0, 64, 2, dtype=np.float32) / 64)).astype(np.float32)
    rc = np.zeros((128, 4), np.float32)
    rc[:, 0] = inv_freq[p % 32] / np.float32(2.0 * np.pi)
    rc[:, 1] = np.where(p % 64 < 32, -1.0, 1.0)
    return rc


def _gains(a, b, c=None, d=None):
    g = np.zeros((128, 48), np.float32)
    if c is not None:
        g[:, 32:38] = np.asarray(c, np.float32).reshape(6, 128).T
    if d is not None:
        g[:, 38:42] = np.asarray(d, np.float32).reshape(4, 128).T
    if a is not None:
        g[:, 0:16] = np.asarray(a, np.float32).reshape(16, 128).T
    if b is not None:
        g[:, 16:32] = np.asarray(b, np.float32).reshape(16, 128).T
    return g


def _run(nc, in_maps):
    res = run_bass_kernel_spmd(nc, in_maps, core_ids=list(range(8)))
    return res.results


def _xchg(res, src_name, dst_name, per):
    out = []
    for c in range(8):
        b, me = c // 4, c % 4
        dd = {}
        for p in range(4):
            for u in range(per):
                dd[f"{dst_name}_{p * per + u}"] = res[b * 4 + p][f"{src_name}_{me * per + u}"]
        out.append(dd)
    return out


def _cat(r, name, n):
    return np.concatenate([r[f"{name}_{i}"] for i in range(n)], axis=0)


_KINDS = ("sb", "hg", "mla")


def _inproj_inputs(inputs, i):
    kind, j = _KINDS[i % 3], i // 3
    if kind == "sb":
        return _wpan_host("w_in", inputs["sb_w_qkv"][j])
    if kind == "hg":
        return _wpan_host("w_in", inputs["hg_w_in"][j])
    wd = np.asarray(inputs["mla_w_dkv"][j], np.float32)
    out = _wpan_host("w_in", wd[:, :1280])
    out.update(_wpan_host("w_kpe", wd[:, 1280:1344]))
    out.update(_wpan_host("w_kpes", np.concatenate([wd[:, 1312:1344], wd[:, 1280:1312]], axis=1)))
    wq = np.asarray(inputs["mla_w_uq"][j], np.float32).reshape(768, 16, 192)
    nope = wq[:, :, :128].reshape(768, 2048)
    rope = wq[:, :, 128:]
    rsw = np.concatenate([rope[:, :, 32:], rope[:, :, :32]], axis=2)
    cols = [nope]
    for r in range(8):
        cols.append(rope[:, 2 * r:2 * r + 2].reshape(768, 128))
        cols.append(rsw[:, 2 * r:2 * r + 2].reshape(768, 128))
    out.update(_wpan_host("w_uq", np.concatenate(cols, axis=1)))
    wk = np.asarray(inputs["mla_w_ukv"][j], np.float32).reshape(512, 16, 256)
    out.update(_wpan_host("w_ukv", np.concatenate([wk[:, :, :128].reshape(512, 2048), wk[:, :, 128:].reshape(512, 2048)], axis=1)))
    return out


def _launch_A(hT, inputs, prev_i, o_in, nxt_i, pos_cores=None):
    n_layers = 4
    kind = "final" if nxt_i >= n_layers else _KINDS[nxt_i % 3]
    cx = Ctx()
    nc = build_phaseA(cx, None if prev_i is None else True, kind)
    norm_mix = np.asarray(inputs["norm_mix"], np.float32)
    norm_mlp = np.asarray(inputs["norm_mlp"], np.float32)
    gm = None if prev_i is None else norm_mlp[prev_i]
    gn = np.asarray(inputs["final_norm"], np.float32) if kind == "final" else norm_mix[nxt_i]
    if kind == "mla":
        g = _gains(gm, gn, inputs["mla_q_norm"][nxt_i // 3], inputs["mla_kv_norm"][nxt_i // 3])
    else:
        g = _gains(gm, gn)
    ws = {}
    if prev_i is not None:
        pk, pj = _KINDS[prev_i % 3], prev_i // 3
        wo = {"sb": "sb_w_o", "hg": "hg_w_o", "mla": "mla_w_o"}[pk]
        ws.update(_wpan_host("w_o", inputs[wo][pj]))
        ws.update(_wpan_host("w_1", inputs["mlp_w1"][prev_i]))
        ws.update(_wpan_host("w_2", inputs["mlp_w2"][prev_i]))
    if kind != "final":
        ws.update(_inproj_inputs(inputs, nxt_i))
    maps = []
    for c in range(8):
        m = dict(hT[c], gains=g, **ws)
        if prev_i is not None:
            m.update(o_in[c])
        if kind == "mla":
            pos = np.asarray(inputs["positions"]).reshape(8, NT)[c].astype(np.int32)
            m["posb"] = np.ascontiguousarray(np.broadcast_to(pos[None, :], (128, NT)))
            m["rconst"] = _rope_consts()
        maps.append(m)
    return _run(nc, maps), kind


def _launch_B(res, kind, inputs=None, layer_i=0):
    per = 4
    recv = _xchg(res, "send", "recv", per)
    cx = Ctx()
    if kind == "sb":
        nc = build_phaseB_sb(cx)
        extra = {"sbc": _sb_consts()}
    elif kind == "mla":
        nc = build_phaseB_mla(cx)
        extra = {"mlc": _mla_consts()}
    else:
        nc = build_phaseB_hg(cx, layer_i)
        extra = {"hgc": _hg_consts(), "gnorm": np.asarray(inputs["hg_g_norm"][layer_i // 3], np.float32).reshape(128, 1)}
    maps = []
    for c in range(8):
        m = dict(recv[c], **extra)
        if kind == "hg":
            g = c % 4
            lt = np.asarray(inputs["hg_lb_logits"], np.float32).T
            m["lbl"] = np.ascontiguousarray(np.concatenate([lt[(4 * g + hh) * 128:(4 * g + hh + 1) * 128] for hh in range(4)], axis=1))
        maps.append(m)
    res = _run(nc, maps)
    return _xchg(res, "sendb", "o_in", 1)


def _h_next(res):
    return [{f"h_in_{q}": r[f"h_out_{q}"] for q in range(4)} for r in res]


def kernel(**inputs):
    x = np.asarray(inputs["x"], np.float32)
    xf = x.reshape(8, NT, D)
    hT = [_chunks_host("h_in", np.ascontiguousarray(xf[c].T), 512) for c in range(8)]
    res, kind = _launch_A(hT, inputs, None, None, 0)
    for i in range(4):
        hT = _h_next(res)
        o_in = _launch_B(res, kind, inputs, i)
        res, kind = _launch_A(hT, inputs, i, o_in, i + 1)
    yT = [_cat(r, "y_out", 4) for r in res]
    out = np.stack([y.T for y in yT], axis=0).reshape(2, S_LEN, D)
    return np.ascontiguousarray(out.astype(np.float32))
```
